# Optimizing a Trainium2 kernel written in Bass

```python
import math
import jax, jax.numpy as jnp
from jax import lax
import numpy as np

D_MODEL = 1024
BATCH = 16
SEQ = 2048
DEPTH = 2

HEAD_DIM = 64
HEADS_PER_GROUP = 4
ATTN_GROUPS = ((128, 1), (512, 4), (2048, 16))
N_ATTN_HEADS = HEADS_PER_GROUP * len(ATTN_GROUPS)
ATTN_WIDTH = N_ATTN_HEADS * HEAD_DIM
ATTN_OUT = HEADS_PER_GROUP * HEAD_DIM
QBLK = 64
POOL_WINDOWS = (2, 4, 8, 16)
POOL_GROUP = D_MODEL // 8
POOL_WIDTH = POOL_GROUP * len(POOL_WINDOWS)
HYENA_WIDTH = D_MODEL // 2
HYENA_ORDER = 2
SHORT_CONV = 3
FILTER_EMB = 33
FILTER_HIDDEN = 64
FILTER_INNER = 2
FILTER_OUT = HYENA_ORDER * 2 * HYENA_WIDTH
DECAY_TARGET = 1e-2
FAST_DECAY_PCT = 0.3
SLOW_DECAY_PCT = 1.5
N_BRANCH = 3
SPLIT_SIZES = (ATTN_WIDTH, ATTN_WIDTH, ATTN_WIDTH, POOL_WIDTH,
               HYENA_WIDTH, HYENA_WIDTH, HYENA_WIDTH, N_BRANCH * D_MODEL)
IN_WIDTH = sum(SPLIT_SIZES)
N_EXPERTS = 32
TOP_K = 4
D_FF = D_MODEL
SWIGLU_LIMIT = 7.0
SWIGLU_ALPHA = 1.702
MOE_BLK = 256
LN_EPS = 1e-5
ALPHA_RES = (2 * DEPTH) ** 0.25
BETA_INIT = (8 * DEPTH) ** -0.25

kernel_name = "hybrid_dilated_pool_hyena_moe_encoder"


def layer_norm(x, g, b):
    xf = x.astype(jnp.float32)
    mu = jnp.mean(xf, axis=-1, keepdims=True)
    var = jnp.mean(jnp.square(xf - mu), axis=-1, keepdims=True)
    return ((xf - mu) * lax.rsqrt(var + LN_EPS) * g + b).astype(x.dtype)


def alibi_slopes():
    h = np.arange(1, N_ATTN_HEADS + 1, dtype=np.float32)
    return jnp.asarray(2.0 ** (-8.0 * h / N_ATTN_HEADS), dtype=jnp.float32)


def dilated_window_attention(q, k, v, slopes, dilation, half):
    B, S, H, hd = q.shape
    L = S // dilation
    Q = math.gcd(QBLK, L)
    nb = L // Q
    kb_len = Q + 2 * half
    qs = q.reshape(B, nb, Q, dilation, H, hd).astype(jnp.float32)
    pad = ((0, 0), (half, half), (0, 0), (0, 0), (0, 0))
    kp = jnp.pad(k.reshape(B, L, dilation, H, hd), pad)
    vp = jnp.pad(v.reshape(B, L, dilation, H, hd), pad)
    idx = jnp.arange(nb)[:, None] * Q + jnp.arange(kb_len)[None, :]
    kb = kp[:, idx].astype(jnp.float32)
    vb = vp[:, idx].astype(jnp.float32)
    s = jnp.einsum('bnqrhd,bnkrhd->bnrhqk', qs, kb) / math.sqrt(hd)
    off = jnp.arange(kb_len)[None, :] - half - jnp.arange(Q)[:, None]
    key_m = idx - half
    valid = (jnp.abs(off) <= half)[None] & ((key_m >= 0) & (key_m < L))[:, None, :]
    bias = -slopes[:, None, None] * (dilation * jnp.abs(off)).astype(jnp.float32)[None]
    s = jnp.where(valid[None, :, None, None], s + bias, -jnp.inf)
    lse = jax.nn.logsumexp(s, axis=-1)
    p = jnp.exp(s - lse[..., None])
    o = jnp.einsum('bnrhqk,bnkrhd->bnqrhd', p, vb).reshape(B, S, H, hd)
    lse = lse.transpose(0, 1, 4, 2, 3).reshape(B, S, H)
    return o, lse


def mixer_attention(q, k, v):
    B, S = q.shape[:2]
    slopes = alibi_slopes()
    outs, lses = [], []
    for g, (window, dilation) in enumerate(ATTN_GROUPS):
        hs = slice(g * HEADS_PER_GROUP, (g + 1) * HEADS_PER_GROUP)
        o, lse = dilated_window_attention(q[:, :, hs], k[:, :, hs], v[:, :, hs], slopes[hs],
                                          dilation, window // (2 * dilation))
        outs.append(o)
        lses.append(lse)
    w = jax.nn.softmax(jnp.stack(lses, axis=0), axis=0)
    o = jnp.einsum('gbsh,gbshd->bshd', w, jnp.stack(outs, axis=0))
    return o.reshape(B, S, ATTN_OUT)


def mixer_pool(y, w_pool, pool_scale):
    B, S, C = y.shape
    yf = y.astype(jnp.float32)
    cs = jnp.concatenate([jnp.zeros((B, 1, C), jnp.float32), jnp.cumsum(yf, axis=1)], axis=1)
    t = jnp.arange(S)
    outs = []
    for g, w in enumerate(POOL_WINDOWS):
        sl = slice(g * POOL_GROUP, (g + 1) * POOL_GROUP)
        lo = jnp.clip(t - w // 2, 0, S)
        hi = jnp.clip(t + w // 2, 0, S)
        mean = (cs[:, hi, sl] - cs[:, lo, sl]) / (hi - lo).astype(jnp.float32)[None, :, None]
        outs.append(mean - yf[..., sl])
    pooled = jnp.stack(outs, axis=2)
    mixed = jnp.einsum('bsgc,gcd->bsgd', pooled, w_pool.astype(jnp.float32)).reshape(B, S, C)
    return (mixed * pool_scale).astype(y.dtype)


def hyena_filters(S, f_w1, f_b1, f_w_inner, f_b_inner, f_w_out, f_freq):
    t = jnp.linspace(0.0, 1.0, S, dtype=jnp.float32)[:, None]
    bands = (FILTER_EMB - 1) // 2
    w = 2.0 * math.pi * jnp.arange(S, dtype=jnp.float32) / S
    f = jnp.linspace(1e-4, bands - 1, bands, dtype=jnp.float32)
    ang = w[:, None] * f[None, :]
    z = jnp.concatenate([t, jnp.cos(ang), -jnp.sin(ang)], axis=-1)
    h = jnp.sin(f_freq * (z @ f_w1 + f_b1))
    for i in range(FILTER_INNER):
        h = jnp.sin(f_freq * (h @ f_w_inner[i] + f_b_inner[i]))
    h = (h @ f_w_out).astype(jnp.float32).reshape(S, HYENA_ORDER, 2, HYENA_WIDTH)
    min_decay = math.log(DECAY_TARGET) / SLOW_DECAY_PCT
    max_decay = math.log(DECAY_TARGET) / FAST_DECAY_PCT
    deltas = jnp.linspace(min_decay, max_decay, HYENA_WIDTH, dtype=jnp.float32)
    decay = jnp.exp(-t * jnp.abs(deltas)[None, :])
    return h * decay[:, None, None, :]


def two_sided_spectrum(h_fwd, h_bwd):
    S, C = h_fwd.shape
    k = jnp.concatenate([h_fwd, jnp.zeros((1, C), jnp.float32), h_bwd[1:][::-1]], axis=0)
    return jnp.fft.rfft(k, axis=0)


def long_conv(z, spec, bias):
    S = z.shape[1]
    zf = jnp.fft.rfft(z, n=2 * S, axis=1)
    y = jnp.fft.irfft(zf * spec[None], n=2 * S, axis=1)[:, :S]
    return y + z * bias


def mixer_hyena(hz, w_sconv, b_sconv, f_w1, f_b1, f_w_inner, f_b_inner, f_w_out, f_freq, hy_bias):
    B, S, C3 = hz.shape
    hz = lax.conv_general_dilated(hz, w_sconv[:, None, :], window_strides=(1,),
                                  padding=((SHORT_CONV // 2, SHORT_CONV // 2),),
                                  dimension_numbers=('NWC', 'WIO', 'NWC'),
                                  feature_group_count=C3) + b_sconv
    v, x1, x2 = jnp.split(hz, 3, axis=-1)
    filt = hyena_filters(S, f_w1, f_b1, f_w_inner, f_b_inner, f_w_out, f_freq)
    z = v.astype(jnp.float32)
    for o, gate in enumerate((x1, x2)):
        spec = two_sided_spectrum(filt[:, o, 0], filt[:, o, 1])
        z = gate.astype(jnp.float32) * long_conv(z, spec, hy_bias[o].astype(jnp.float32))
    return z.astype(hz.dtype)


def hybrid_mixer(u, w_in, w_sconv, b_sconv, w_pool, pool_scale, f_w1, f_b1, f_w_inner, f_b_inner,
                 f_w_out, f_freq, hy_bias, p_attn, p_pool, p_hyena, w_out):
    B, S, D = u.shape
    proj = u @ w_in
    cuts = np.cumsum(SPLIT_SIZES)[:-1].tolist()
    q, k, v, yp, hv, hx1, hx2, g_logit = jnp.split(proj, cuts, axis=-1)
    heads = lambda t: t.reshape(B, S, N_ATTN_HEADS, HEAD_DIM)
    ya = mixer_attention(heads(q), heads(k), heads(v)).astype(u.dtype)
    yb = mixer_pool(yp, w_pool, pool_scale)
    yc = mixer_hyena(jnp.concatenate([hv, hx1, hx2], axis=-1), w_sconv, b_sconv, f_w1, f_b1,
                     f_w_inner, f_b_inner, f_w_out, f_freq, hy_bias)
    g = jax.nn.sigmoid(g_logit.astype(jnp.float32)).reshape(B, S, N_BRANCH, D)
    merged = g[:, :, 0] * (ya @ p_attn) + g[:, :, 1] * (yb @ p_pool) + g[:, :, 2] * (yc @ p_hyena)
    return merged.astype(u.dtype) @ w_out


def moe_ffn(u, w_router, b_router, w1, b1, w2, b2):
    B, S, D = u.shape
    N = B * S
    NK = N * TOP_K
    xt = u.reshape(N, D)
    logits = (xt @ w_router + b_router).astype(jnp.float32)
    top_v, top_e = lax.top_k(logits, TOP_K)
    gates = jax.nn.softmax(top_v, axis=-1)
    flat_e = top_e.reshape(NK)
    order = jnp.argsort(flat_e)
    sorted_e = flat_e[order]
    counts = jnp.bincount(flat_e, length=N_EXPERTS)
    padded = (counts + MOE_BLK - 1) // MOE_BLK * MOE_BLK
    pend = jnp.cumsum(padded)
    cend = jnp.cumsum(counts)
    dest = (pend - padded)[sorted_e] + jnp.arange(NK) - (cend - counts)[sorted_e]
    n_blocks = -(-NK // MOE_BLK) + N_EXPERTS
    R = n_blocks * MOE_BLK
    row_tok = jnp.zeros((R,), jnp.int32).at[dest].set((order // TOP_K).astype(jnp.int32))
    row_gate = jnp.zeros((R,), jnp.float32).at[dest].set(gates.reshape(NK)[order])
    blk_e = jnp.minimum(jnp.searchsorted(pend, jnp.arange(n_blocks) * MOE_BLK, side='right'),
                        N_EXPERTS - 1)

    def expert_block(args):
        tok, gate, e = args
        h = xt[tok] @ w1[e] + b1[e]
        glu, lin = jnp.split(h, 2, axis=-1)
        glu = jnp.minimum(glu, SWIGLU_LIMIT)
        lin = jnp.clip(lin, -SWIGLU_LIMIT, SWIGLU_LIMIT)
        act = glu * jax.nn.sigmoid(SWIGLU_ALPHA * glu) * (lin + 1.0)
        y = act @ w2[e] + b2[e]
        return y * gate[:, None].astype(y.dtype)

    ys = lax.map(expert_block, (row_tok.reshape(n_blocks, MOE_BLK),
                                row_gate.reshape(n_blocks, MOE_BLK), blk_e))
    out = jax.ops.segment_sum(ys.reshape(R, D), row_tok, num_segments=N)
    return out.reshape(B, S, D).astype(u.dtype)


def setup_inputs(seed: int = 0) -> dict:
    key = jax.random.key(seed)
    ks = jax.random.split(key, 32)
    f32 = jnp.float32
    nrm = lambda k, shape, std: jax.random.normal(k, shape, f32) * std
    L, D, C = DEPTH, D_MODEL, HYENA_WIDTH
    return {
        "x": nrm(ks[0], (BATCH, SEQ, D), 1.0),
        "c": nrm(ks[1], (BATCH, D), 1.0),
        "w_ada": nrm(ks[2], (L, D, 6 * D), 0.2 * D ** -0.5),
        "b_ada": nrm(ks[3], (L, 6 * D), 0.01),
        "w_in": nrm(ks[4], (L, D, IN_WIDTH), D ** -0.5),
        "w_sconv": nrm(ks[5], (L, SHORT_CONV, 3 * C), SHORT_CONV ** -0.5),
        "b_sconv": nrm(ks[6], (L, 3 * C), 0.01),
        "w_pool": nrm(ks[7], (L, len(POOL_WINDOWS), POOL_GROUP, POOL_GROUP), POOL_GROUP ** -0.5),
        "pool_scale": 1.0 + nrm(ks[8], (L, POOL_WIDTH), 0.02),
        "f_w1": nrm(ks[9], (L, FILTER_EMB, FILTER_HIDDEN), FILTER_EMB ** -0.5),
        "f_b1": nrm(ks[10], (L, FILTER_HIDDEN), 0.02),
        "f_w_inner": nrm(ks[11], (L, FILTER_INNER, FILTER_HIDDEN, FILTER_HIDDEN), FILTER_HIDDEN ** -0.5),
        "f_b_inner": nrm(ks[12], (L, FILTER_INNER, FILTER_HIDDEN), 0.02),
        "f_w_out": nrm(ks[13], (L, FILTER_HIDDEN, FILTER_OUT), 0.01),
        "f_freq": 1.0 + nrm(ks[14], (L, FILTER_HIDDEN), 0.02),
        "hy_bias": nrm(ks[15], (L, HYENA_ORDER, C), 1.0),
        "p_attn": nrm(ks[16], (L, ATTN_OUT, D), ATTN_OUT ** -0.5),
        "p_pool": nrm(ks[17], (L, POOL_WIDTH, D), POOL_WIDTH ** -0.5),
        "p_hyena": nrm(ks[18], (L, C, D), C ** -0.5),
        "w_out": nrm(ks[19], (L, D, D), BETA_INIT * D ** -0.5),
        "ln1_g": 1.0 + nrm(ks[20], (L, D), 0.02),
        "ln1_b": nrm(ks[21], (L, D), 0.02),
        "w_router": nrm(ks[22], (L, D, N_EXPERTS), D ** -0.5),
        "b_router": nrm(ks[23], (L, N_EXPERTS), 0.01),
        "w1": nrm(ks[24], (L, N_EXPERTS, D, 2 * D_FF), D ** -0.5),
        "b1": nrm(ks[25], (L, N_EXPERTS, 2 * D_FF), 0.01),
        "w2": nrm(ks[26], (L, N_EXPERTS, D_FF, D), BETA_INIT * D_FF ** -0.5),
        "b2": nrm(ks[27], (L, N_EXPERTS, D), 0.01),
        "ln2_g": 1.0 + nrm(ks[28], (L, D), 0.02),
        "ln2_b": nrm(ks[29], (L, D), 0.02),
    }


def reference(x, c, w_ada, b_ada, w_in, w_sconv, b_sconv, w_pool, pool_scale, f_w1, f_b1, f_w_inner,
              f_b_inner, f_w_out, f_freq, hy_bias, p_attn, p_pool, p_hyena, w_out, ln1_g, ln1_b,
              w_router, b_router, w1, b1, w2, b2, ln2_g, ln2_b):
    B, S, D = x.shape
    cond = jax.nn.silu(c)
    for l in range(DEPTH):
        mod = (cond @ w_ada[l] + b_ada[l]).reshape(B, 6, 1, D)
        shift1, scale1, gate1 = mod[:, 0], mod[:, 1], mod[:, 2]
        shift2, scale2, gate2 = mod[:, 3], mod[:, 4], mod[:, 5]
        u = x * (1.0 + scale1) + shift1
        h = hybrid_mixer(u, w_in[l], w_sconv[l], b_sconv[l], w_pool[l], pool_scale[l], f_w1[l], f_b1[l],
                         f_w_inner[l], f_b_inner[l], f_w_out[l], f_freq[l], hy_bias[l],
                         p_attn[l], p_pool[l], p_hyena[l], w_out[l])
        x = layer_norm(ALPHA_RES * x + (1.0 + gate1) * h, ln1_g[l], ln1_b[l])
        u = x * (1.0 + scale2) + shift2
        h = moe_ffn(u, w_router[l], b_router[l], w1[l], b1[l], w2[l], b2[l])
        x = layer_norm(ALPHA_RES * x + (1.0 + gate2) * h, ln2_g[l], ln2_b[l])
    return x
```

```python
import math
from contextlib import ExitStack
import numpy as np
import ml_dtypes
import concourse.bass as bass
import concourse.mybir as mybir
from concourse.bass_utils import run_bass_kernel_spmd

F32 = mybir.dt.float32
BF16 = mybir.dt.bfloat16
I32 = mybir.dt.int32
AF = mybir.ActivationFunctionType
ALU = mybir.AluOpType
AX = mybir.AxisListType

D = 1024
S = 2048
NB = 2
T = NB * S
DEPTH = 2
NE = 32
CAP = 896
NROWS = NE * CAP
IN_W = 7424
C_Q, C_K, C_V, C_P, C_HV, C_G = 0, 768, 1536, 2304, 2816, 4352
GROUPS = ((128, 1), (512, 4), (2048, 16))
ALPHA = (2 * DEPTH) ** 0.25
EPS = 1e-5
BIG = float(NROWS)


class Buf:
    __slots__ = ("name", "last_w", "readers", "dram")

    def __init__(self, name):
        self.name = name
        self.last_w = None
        self.readers = []
        self.dram = False


class Sched:
    COMPUTE = ("pe", "dve", "act", "pool")
    NRING = 24

    def __init__(self, nc):
        self.nc = nc
        self.ops = []

    def buf(self, name="b"):
        return Buf(name)

    def op(self, eng, fn, reads=(), writes=(), dma=False):
        reads = tuple(b for b in reads if not getattr(b, "dram", False))
        writes = tuple(b for b in writes if not getattr(b, "dram", False))
        self.ops.append((eng, fn, reads, writes, dma))

    def dma(self, q, out, in_, reads=(), writes=()):
        self.op(q, lambda e: e.dma_start(out=out, in_=in_), reads, writes, dma=True)

    def barrier(self):
        self.ops.append(("sp", "BARRIER", (), (), False))

    def mark(self, name):
        if not hasattr(self, "marks"):
            self.marks = []
        cnt = {}
        for o in self.ops:
            if o[1] not in (None, "BARRIER"):
                cnt[o[0]] = cnt.get(o[0], 0) + 1
        self.marks.append((name, cnt))

    def finalize(self, final_wait_bufs=()):
        nc = self.nc
        ops = self.ops
        ops.append(("sp", None, tuple(final_wait_bufs), (), False))
        n = len(ops)
        deps = [None] * n
        has_dep = [False] * n
        last_eng = {}
        dmas_since = []
        extra = {}
        exp_ops = []
        for (eng, fn, reads, writes, dma) in ops:
            if fn == "BARRIER":
                exp_ops.append(("sp", "BAR0", (), (), False))
                for e2 in ("pe", "dve", "act", "pool"):
                    exp_ops.append((e2, "BAR1", (), (), False))
            else:
                exp_ops.append((eng, fn, reads, writes, dma))
        ops = exp_ops
        n = len(ops)
        deps = [None] * n
        has_dep = [False] * n
        bar0 = None
        for i, (eng, fn, reads, writes, dma) in enumerate(ops):
            if fn == "BAR0":
                dl = sorted(set(list(last_eng.values()) + dmas_since))
                deps[i] = [p for p in dl]
                for p in deps[i]:
                    has_dep[p] = True
                last_eng = {}
                dmas_since = []
                bar0 = i
                continue
            if fn == "BAR1":
                deps[i] = [bar0]
                has_dep[bar0] = True
                continue
            d = set()
            for b in reads:
                if b.last_w is not None:
                    d.add((b.last_w, 0))
            for b in writes:
                if b.last_w is not None:
                    d.add((b.last_w, 1))
                for r in b.readers:
                    d.add((r, 2))
            for b in reads:
                b.readers.append(i)
            for b in writes:
                b.last_w = i
                b.readers = []
            need = set()
            for (p, kind) in d:
                if p == i:
                    continue
                peng, _, _, _, pdma = ops[p]
                if (not pdma) and peng == eng and (not dma):
                    if eng == "pe":
                        continue
                    if kind != 0:
                        continue
                need.add(p)
            best = {}
            nn = []
            for p in need:
                if ops[p][4]:
                    nn.append(p)
                else:
                    pe_ = ops[p][0]
                    if best.get(pe_, -1) < p:
                        best[pe_] = p
            deps[i] = sorted(nn + list(best.values()))
            for p in deps[i]:
                has_dep[p] = True
            if dma:
                dmas_since.append(i)
            else:
                last_eng[eng] = i
        tick = {e: 0 for e in self.COMPUTE + ("sp",)}
        dcount = {}
        sig = [None] * n
        for i, (eng, fn, reads, writes, dma) in enumerate(ops):
            if dma:
                j = dcount.get(eng, 0)
                dcount[eng] = j + 1
                sig[i] = ("dma", (eng, j % self.NRING), 16 * (j // self.NRING + 1), j)
            elif has_dep[i]:
                tick[eng] += 1
                sig[i] = ("eng", eng, tick[eng], None)
        with ExitStack() as st:
            sems = {}
            for e in self.COMPUTE + ("sp",):
                if tick[e] > 0:
                    sems[("eng", e)] = st.enter_context(nc.semaphore(f"s_{e}"))
            for q, cnt in dcount.items():
                for r in range(min(cnt, self.NRING)):
                    sems[("dma", (q, r))] = st.enter_context(nc.semaphore(f"d_{q}{r}"))
            block = st.enter_context(nc.Block())
            per_eng = {}
            for i, o in enumerate(ops):
                per_eng.setdefault(o[0], []).append(i)
            nw = [0]

            def emit_engine(ename, e):
                waited = {}
                if ename in getattr(self, "prologue", {}):
                    self.prologue[ename](e)
                for i in per_eng.get(ename, []):
                    eng, fn, reads, writes, dma = ops[i]
                    wl = {}
                    for p in deps[i]:
                        k, key, val, _ = sig[p]
                        sk = (k, key)
                        if waited.get(sk, 0) >= val:
                            continue
                        if wl.get(sk, 0) < val:
                            wl[sk] = val
                    if dma:
                        k, key, val, j = sig[i]
                        if j >= self.NRING:
                            sk = (k, key)
                            pv = val - 16
                            if waited.get(sk, 0) < pv and wl.get(sk, 0) < pv:
                                wl[sk] = pv
                    for sk, val in wl.items():
                        e.wait_ge(sems[sk], val)
                        waited[sk] = val
                        nw[0] += 1
                    if fn is None:
                        continue
                    if fn in ("BAR0", "BAR1"):
                        ins = e.nop()
                    else:
                        ins = fn(e)
                    if sig[i] is not None:
                        k, key, val, _ = sig[i]
                        ins.then_inc(sems[(k, key)], 16 if k == "dma" else 1)

            name2dec = {"pe": block.tensor, "dve": block.vector, "act": block.scalar,
                        "pool": block.gpsimd, "sp": block.sync}
            for ename, dec in name2dec.items():
                if ename in per_eng:
                    dec(lambda e, ename=ename: emit_engine(ename, e))
        self.stats = dict(n_ops=n, ticks=tick, dmas=dcount, waits=nw[0])


class Arena:
    def __init__(self, nc, nbytes):
        self.t = nc.alloc_sbuf_tensor("arena", [128, nbytes // 4], F32)
        self.nbytes = nbytes
        self.off = 0
        self.base = 0

    def alloc(self, free_shape, dtype=F32):
        esz = 2 if dtype == BF16 else 4
        n = 1
        for s in free_shape:
            n *= s
        nb = (n * esz + 63) // 64 * 64
        assert self.off + nb <= self.nbytes, f"arena overflow {self.off}+{nb}>{self.nbytes}"
        a = self.t[:, self.off // 4:(self.off + nb) // 4]
        self.off += nb
        if dtype != F32:
            a = a.bitcast(dtype)
        a = a[:, 0:n]
        if len(free_shape) == 2:
            a = a.rearrange("p (a b) -> p a b", b=free_shape[1])
        elif len(free_shape) == 3:
            a = a.rearrange("p (a b c) -> p a b c", b=free_shape[1], c=free_shape[2])
        return a

    def mark_persistent(self):
        self.base = self.off

    def reset(self):
        self.off = self.base


def host_constants():
    N = 2 * S
    t = np.arange(S, dtype=np.float64)
    f = np.arange(S, dtype=np.float64)
    ang = 2.0 * np.pi * np.outer(t, f) / N
    Fm = np.zeros((S, N), np.float64)
    Fm[:, 0:S] = np.cos(ang)
    Fm[:, S + 1:] = -np.sin(ang[:, 1:])
    Fm[:, S] = np.cos(np.pi * t)
    Fh = Fm.reshape(16, 128, 32, 128).transpose(2, 1, 0, 3)
    dftF = np.ascontiguousarray(Fh).astype(ml_dtypes.bfloat16)
    Gm = np.zeros((N, S), np.float64)
    Gm[0:S, :] = (2.0 / N) * np.cos(ang.T)
    Gm[0, :] = 1.0 / N
    Gm[S + 1:, :] = -(2.0 / N) * np.sin(ang.T[1:, :])
    Gm[S, :] = (1.0 / N) * np.cos(np.pi * t)
    Gh = Gm.reshape(32, 128, 16, 128).transpose(2, 1, 0, 3)
    dftG = np.ascontiguousarray(Gh).astype(ml_dtypes.bfloat16)
    tt = np.linspace(0.0, 1.0, S, dtype=np.float32)[:, None]
    bands = 16
    w = (2.0 * math.pi * np.arange(S, dtype=np.float32) / S).astype(np.float32)
    fr = np.linspace(1e-4, bands - 1, bands, dtype=np.float32)
    angz = (w[:, None] * fr[None, :]).astype(np.float32)
    z = np.concatenate([tt, np.cos(angz), -np.sin(angz)], axis=-1).astype(np.float32)
    zT = np.ascontiguousarray(z.T)
    min_decay = math.log(1e-2) / 1.5
    max_decay = math.log(1e-2) / 0.3
    deltas = np.linspace(min_decay, max_decay, 512, dtype=np.float32)
    decay = np.exp(-tt * np.abs(deltas)[None, :]).astype(np.float32)
    h = np.arange(1, 13, dtype=np.float32)
    slopes = (2.0 ** (-8.0 * h / 12)).astype(np.float32)
    row = np.arange(128)[:, None]
    col = np.arange(256)[None, :]
    dist = np.abs(row - col + 64).astype(np.float32)
    am = np.zeros((128, 12, 256), np.float32)
    for hd in range(12):
        dil = GROUPS[hd // 4][1]
        am[:, hd, :] = np.where(dist <= 64, np.exp(-slopes[hd] * dil * dist), 0.0)
    tpos = np.arange(S)
    ic = np.zeros((4, S), np.float32)
    for g, wd in enumerate((2, 4, 8, 16)):
        lo = np.clip(tpos - wd // 2, 0, S)
        hi = np.clip(tpos + wd // 2, 0, S)
        ic[g] = 1.0 / (hi - lo).astype(np.float32)
    ident = np.eye(128, dtype=np.float32)
    ltri = (np.arange(128)[:, None] < np.arange(128)[None, :]).astype(np.float32)
    eoff = np.tile((np.arange(NE, dtype=np.float32) * CAP)[None, :], (128, 1))
    return dict(dftF=dftF, dftG=dftG, zT=zT, decay=decay, amask=am, invcnt=ic, ident=ident,
                ltri=ltri, eoff=eoff)


WEIGHT_NAMES = ["w_ada", "b_ada", "w_in", "w_sconv", "b_sconv", "w_pool", "pool_scale", "f_w1", "f_b1",
                "f_w_inner", "f_b_inner", "f_w_out", "f_freq", "hy_bias", "p_attn", "p_pool", "p_hyena",
                "w_out", "ln1_g", "ln1_b", "w_router", "b_router", "w1", "b1", "w2", "b2", "ln2_g", "ln2_b"]


INPUT_SHAPES = {
    "x": [NB, S, D], "c": [NB, D], "w_ada": [2, D, 6 * D], "b_ada": [2, 6 * D], "w_in": [2, D, IN_W],
    "w_sconv": [2, 3, 1536], "b_sconv": [2, 1536], "w_pool": [2, 4, 128, 128], "pool_scale": [2, 512],
    "f_w1": [2, 33, 64], "f_b1": [2, 64], "f_w_inner": [2, 2, 64, 64], "f_b_inner": [2, 2, 64],
    "f_w_out": [2, 64, 2048], "f_freq": [2, 64], "hy_bias": [2, 2, 512], "p_attn": [2, 256, D],
    "p_pool": [2, 512, D], "p_hyena": [2, 512, D], "w_out": [2, D, D], "ln1_g": [2, D], "ln1_b": [2, D],
    "w_router": [2, D, NE], "b_router": [2, NE], "w1": [2, NE, D, 2 * D], "b1": [2, NE, 2 * D],
    "w2": [2, NE, D, D], "b2": [2, NE, D], "ln2_g": [2, D], "ln2_b": [2, D],
}
CONST_SHAPES = {
    "dftF": ([32, 128, 16, 128], BF16), "dftG": ([16, 128, 32, 128], BF16), "zT": ([33, S], F32),
    "decay": ([S, 512], F32), "amask": ([128, 12, 256], F32), "invcnt": ([4, S], F32),
    "ident": ([128, 128], F32), "ltri": ([128, 128], F32), "eoff": ([128, NE], F32),
}


def build_program(debug=(), n_layers=DEPTH, stop_after=None):
    nc = bass.Bass("TRN2", target_bir_lowering=False)
    IN = {k: nc.dram_tensor(k, shp, F32, kind="ExternalInput").ap() for k, shp in INPUT_SHAPES.items()}
    CN = {k: nc.dram_tensor(k, shp, dt, kind="ExternalInput").ap() for k, (shp, dt) in CONST_SHAPES.items()}
    out_d = nc.dram_tensor("out", [NB, S, D], F32, kind="ExternalOutput").ap()

    def scratch(name, shape, dt=F32):
        kind = "ExternalOutput" if name in debug else "Internal"
        return nc.dram_tensor(name, shape, dt, kind=kind).ap()

    modd = scratch("modd", [2, NB, 6 * D])
    kspec = scratch("kspec", [32, 128, 1024])
    uTd = scratch("uTd", [NB, 128, 8, S], BF16)
    cTd = scratch("cTd", [3, 16, 128, 1024], BF16)
    yaTd = scratch("yaTd", [NB, 64, 4, S], BF16)
    ybTd = scratch("ybTd", [NB, 128, 4, S], BF16)
    ycTd = scratch("ycTd", [NB, 128, 4, S], BF16)
    x1d = scratch("x1d", [T, D])
    xcur = scratch("xcur", [T, D])
    xe_d = scratch("xe_d", [NROWS + 128, D], BF16)
    ye_d = scratch("ye_d", [NROWS + 128, D], BF16)
    dbg_log = scratch("dbg_log", [T, NE])
    dbg_dest = scratch("dbg_dest", [T, 4])

    SC = Sched(nc)
    holder = {}

    def pool_prologue(e):
        reg = e.alloc_register("bcreg")
        e.reg_mov(reg, NROWS)
        holder["bc"] = reg
    SC.prologue = {"pool": pool_prologue}
    AR = Arena(nc, 200 * 1024)
    B = SC.buf
    db = {n: B(n) for n in ["modd", "kspec", "uTd", "cTd", "yaTd", "ybTd", "ycTd", "x1d", "xcur", "xe_d",
                            "ye_d", "out", "dbg"]}
    for b_ in db.values():
        b_.dram = True

    PSB = [nc.alloc_psum_tensor(f"ps{i}", [128, 512], F32)[:] for i in range(8)]
    PSBUF = [B(f"ps{i}") for i in range(8)]
    psi = [0]

    def nextps():
        i = psi[0] % 8
        psi[0] += 1
        return PSB[i], PSBUF[i]

    class Pool_:
        def __init__(self, shape, dtype, n):
            self.t = [AR.alloc(shape, dtype) for _ in range(n)]
            self.b = [B("pl") for _ in range(n)]
            self.i = 0

        def next(self):
            i = self.i % len(self.t)
            self.i += 1
            return self.t[i], self.b[i]

    def mm(ps, lhsT, rhs, start, stop, reads, wb):
        SC.op("pe", lambda e: e.matmul(ps, lhsT=lhsT, rhs=rhs, start=start, stop=stop), reads, [wb])

    def tr(ps, in_, ident, reads, wb):
        SC.op("pe", lambda e: e.transpose(ps, in_, ident), reads, [wb])

    def act(out, in_, func, reads, writes, bias=None, scale=None):
        kw = {}
        if bias is not None:
            kw["bias"] = bias
        if scale is not None:
            kw["scale"] = scale
        SC.op("act", lambda e: e.activation(out=out, in_=in_, func=func, **kw), reads, writes)

    def cp(eng, out, in_, reads, writes):
        if eng == "act":
            act(out, in_, AF.Copy, reads, writes)
        else:
            SC.op(eng, lambda e: e.tensor_copy(out=out, in_=in_), reads, writes)

    def tt(eng, out, in0, in1, op, reads, writes):
        SC.op(eng, lambda e: e.tensor_tensor(out=out, in0=in0, in1=in1, op=op), reads, writes)

    def ts(eng, out, in0, s1, s2, op0, op1, reads, writes):
        if op1 is None:
            SC.op(eng, lambda e: e.tensor_scalar(out=out, in0=in0, scalar1=s1, scalar2=None, op0=op0), reads, writes)
        else:
            SC.op(eng, lambda e: e.tensor_scalar(out=out, in0=in0, scalar1=s1, scalar2=s2, op0=op0, op1=op1), reads, writes)

    def stt(eng, out, in0, scalar, in1, op0, op1, reads, writes):
        SC.op(eng, lambda e: e.scalar_tensor_tensor(out=out, in0=in0, scalar=scalar, in1=in1, op0=op0, op1=op1),
              reads, writes)

    def memset(eng, ap, val, writes):
        SC.op(eng, lambda e: e.memset(ap, val), (), writes)

    def dma(q, out, in_, reads, writes):
        SC.dma(q, out, in_, reads, writes)

    def dma_nc(q, out, in_, reads, writes):
        def f(e):
            with nc.allow_non_contiguous_dma(reason="small param layout"):
                return e.dma_start(out=out, in_=in_)
        SC.op(q, f, reads, writes, dma=True)

    ident_f = AR.alloc([128]); ident_b = AR.alloc([128], BF16)
    ltri = AR.alloc([128]); ones_f = AR.alloc([128]); eoff = AR.alloc([NE]); epsb = AR.alloc([1])
    dest_all = nc.alloc_sbuf_tensor("dest_all", [128, (T // 128) * 4], I32); gate_all = AR.alloc([T // 128, 4])
    cb = B("consts")
    dma("sp", ident_f, CN["ident"], (), [cb])
    dma("sp", ltri, CN["ltri"], (), [cb])
    dma("sp", eoff, CN["eoff"], (), [cb])
    cp("dve", ident_b, ident_f, [cb], [cb])
    memset("dve", ones_f, 1.0, [cb])
    memset("dve", epsb, EPS, [cb])
    b_route = B("route")
    b_route_t = [B(f"route{i}") for i in range(T // 128)]
    AR.mark_persistent()

    def phase_ada():
        AR.reset()
        cT = AR.alloc([NB, 8]); condT = AR.alloc([NB, 8])
        bct = B("cT")
        for b_ in range(NB):
            dma_nc("sp", cT[:, b_, :], IN["c"][b_].rearrange("(k p) -> p k", p=128), (), [bct])
        act(condT, cT, AF.Silu, [bct], [bct])
        wpool = Pool_([8, 512], F32, 2)
        modsb = AR.alloc([6 * D]); bsb = AR.alloc([6 * D])
        bm = B("modsb")
        for l in range(2):
            bb = B("bsb")
            dma("sp", bsb[0:NB, :], IN["b_ada"][l].partition_broadcast(NB), [bm], [bb])
            for nb_ in range(12):
                wt, wbuf = wpool.next()
                dma("sp", wt, IN["w_ada"][l].rearrange("(k p) n -> p k n", p=128)[:, :, nb_ * 512:(nb_ + 1) * 512],
                    (), [wbuf])
                ps, pb = nextps()
                for k in range(8):
                    mm(ps[0:NB, :], condT[:, :, k], wt[:, k, :], k == 0, k == 7, [bct, wbuf], pb)
                tt("dve", modsb[0:NB, nb_ * 512:(nb_ + 1) * 512], ps[0:NB, :], bsb[0:NB, nb_ * 512:(nb_ + 1) * 512],
                   ALU.add, [pb, bb], [bm])
            for sec in (1, 2, 4, 5):
                ts("dve", modsb[0:NB, sec * D:(sec + 1) * D], modsb[0:NB, sec * D:(sec + 1) * D], 1.0, None,
                   ALU.add, None, [bm], [bm])
            dma("sp", modd[l], modsb[0:NB, :], [bm], [db["modd"]])

    def load_rep(dst, src_row, reads, wbuf, q="sp"):
        dma(q, dst, src_row.partition_broadcast(128), reads, [wbuf])

    def phase_filter(l):
        AR.reset()
        zT = AR.alloc([S]); bz = B("zT")
        dma("sp", zT[0:33, :], CN["zT"], (), [bz])
        w1 = AR.alloc([64]); wi = AR.alloc([2, 64]); wo = AR.alloc([2048]); prm = AR.alloc([8]); bw = B("fw")
        dma("sp", w1[0:33, :], IN["f_w1"][l], (), [bw])
        dma_nc("sp", wi[0:64], IN["f_w_inner"][l].rearrange("i k m -> k i m"), (), [bw])
        dma("sp", wo[0:64, :], IN["f_w_out"][l], (), [bw])
        dma_nc("sp", prm[0:64, 0:1], IN["f_freq"][l].rearrange("(p o) -> p o", o=1), (), [bw])
        dma_nc("sp", prm[0:64, 1:2], IN["f_b1"][l].rearrange("(p o) -> p o", o=1), (), [bw])
        dma_nc("sp", prm[0:64, 2:4], IN["f_b_inner"][l].rearrange("i p -> p i"), (), [bw])
        for j in range(3):
            tt("dve", prm[0:64, 4 + j:5 + j], prm[0:64, 1 + j:2 + j], prm[0:64, 0:1], ALU.mult, [bw], [bw])
        hA = AR.alloc([S]); hB = AR.alloc([S]); tmp = AR.alloc([S])
        bh = [B("hA"), B("hB")]; btmp = B("tmp")
        hs = [hA, hB]
        src = zT; bsrc = bz; ksz = 33
        for layer in range(3):
            dst = hs[layer % 2]; bdst = bh[layer % 2]
            lw = w1[0:33, :] if layer == 0 else wi[0:64, layer - 1, :]
            for tb in range(4):
                ps, pb = nextps()
                mm(ps[0:64, :], lw, src[0:ksz, tb * 512:(tb + 1) * 512], True, True, [bw, bsrc], pb)
                act(dst[0:64, tb * 512:(tb + 1) * 512], ps[0:64, :], AF.Identity, [pb, bw], [bdst],
                    bias=prm[0:64, 4 + layer:5 + layer], scale=prm[0:64, 0:1])
            ts("dve", tmp[0:64, :], dst[0:64, :], math.pi, -2.0 * math.pi, ALU.is_gt, ALU.mult, [bdst], [btmp])
            tt("dve", tmp[0:64, :], tmp[0:64, :], dst[0:64, :], ALU.add, [btmp, bdst], [btmp])
            ts("dve", dst[0:64, :], dst[0:64, :], -math.pi, 2.0 * math.pi, ALU.is_lt, ALU.mult, [bdst, btmp], [bdst])
            tt("dve", dst[0:64, :], dst[0:64, :], tmp[0:64, :], ALU.add, [bdst, btmp], [bdst])
            act(dst[0:64, :], dst[0:64, :], AF.Sin, [bdst], [bdst])
            src = dst; bsrc = bdst; ksz = 64
        h3 = src; bh3 = bsrc
        hsum = AR.alloc([16, 1024], BF16); hdiff = AR.alloc([16, 1024], BF16)
        bhs = B("hsum")
        dec = Pool_([512], F32, 2)
        hf = Pool_([2048], F32, 2)
        for tc in range(16):
            dt_, dbf = dec.next()
            dma("sp", dt_, CN["decay"][tc * 128:(tc + 1) * 128, :], (), [dbf])
            ht, hb = hf.next()
            for cb4 in range(4):
                ps, pb = nextps()
                mm(ps, h3[0:64, tc * 128:(tc + 1) * 128], wo[0:64, cb4 * 512:(cb4 + 1) * 512], True, True,
                   [bh3, bw], pb)
                tt("dve", ht[:, cb4 * 512:(cb4 + 1) * 512], ps, dt_, ALU.mult, [pb, dbf], [hb])
            if tc == 0:
                for o in range(2):
                    memset("dve", ht[0:1, (2 * o + 1) * 512:(2 * o + 2) * 512], 0.0, [hb])
            for o in range(2):
                tt("pool", hsum[:, tc, o * 512:(o + 1) * 512], ht[:, (2 * o) * 512:(2 * o + 1) * 512],
                   ht[:, (2 * o + 1) * 512:(2 * o + 2) * 512], ALU.add, [hb], [bhs])
                tt("pool", hdiff[:, tc, o * 512:(o + 1) * 512], ht[:, (2 * o) * 512:(2 * o + 1) * 512],
                   ht[:, (2 * o + 1) * 512:(2 * o + 2) * 512], ALU.subtract, [hb], [bhs])
        brep = AR.alloc([1024]); bbr = B("brep")
        load_rep(brep, IN["hy_bias"][l].rearrange("o c -> (o c)"), (), bbr)
        fpool = Pool_([16, 128], BF16, 3)
        kout = Pool_([1024], F32, 3)
        def load_f(fc):
            ft, fb = fpool.next()
            dma("sp", ft, CN["dftF"][fc], (), [fb])
            return ft, fb
        fq = [load_f(0), load_f(1)]
        for fc in range(32):
            ft, fb = fq.pop(0)
            if fc + 2 < 32:
                fq.append(load_f(fc + 2))
            src_h = hsum if fc < 16 else hdiff
            kt_, kb = kout.next()
            pss = []
            for hh in range(2):
                ps, pb = nextps()
                for tc in range(16):
                    mm(ps, ft[:, tc, :], src_h[:, tc, hh * 512:(hh + 1) * 512], tc == 0, tc == 15, [fb, bhs], pb)
                pss.append((ps, pb))
            for hh in range(2):
                ps, pb = pss[hh]
                if fc < 16:
                    tt("dve", kt_[:, hh * 512:(hh + 1) * 512], ps, brep[:, hh * 512:(hh + 1) * 512], ALU.add,
                       [pb, bbr], [kb])
                else:
                    cp("act", kt_[:, hh * 512:(hh + 1) * 512], ps, [pb], [kb])
            if fc == 16:
                for hh in range(2):
                    ps, pb = nextps()
                    for tc in range(16):
                        mm(ps[0:1, :], ft[:, tc, 0:1], hsum[:, tc, hh * 512:(hh + 1) * 512], tc == 0, tc == 15,
                           [fb, bhs], pb)
                    tt("dve", kt_[0:1, hh * 512:(hh + 1) * 512], ps[0:1, :], brep[0:1, hh * 512:(hh + 1) * 512],
                       ALU.add, [pb, bbr, kb], [kb])
            dma("sp", kspec[fc], kt_, [kb], [db["kspec"]])

    def load_w_block(dst, wsrc, col0, ncols, wbuf, reads=()):
        dma("pool", dst, wsrc.rearrange("(c p) n -> p c n", p=128)[:, :, col0:col0 + ncols], reads, [wbuf])

    def phase_seq_front(l, s):
        AR.reset()
        xin = IN["x"][s] if l == 0 else xcur[s * S:(s + 1) * S, :]
        xin_reads = [] if l == 0 else [db["xcur"]]
        uT = AR.alloc([8, S], BF16); buT = B("uT")
        sc1 = AR.alloc([D]); sh1 = AR.alloc([D]); bmod = B("mod")
        load_rep(sh1, modd[l, s, 0:D], [db["modd"]], bmod)
        load_rep(sc1, modd[l, s, D:2 * D], [db["modd"]], bmod)
        mark = AR.off
        xp = Pool_([D], F32, 2); up = Pool_([D], BF16, 2); ut = Pool_([D], F32, 2)
        for tI in range(16):
            xt, xb = xp.next()
            dma("sp", xt, xin[tI * 128:(tI + 1) * 128, :], xin_reads, [xb])
            u32, u32b = ut.next()
            tt("dve", u32, xt, sc1, ALU.mult, [xb, bmod], [u32b])
            ub, ubb = up.next()
            tt("dve", ub, u32, sh1, ALU.add, [u32b, bmod], [ubb])
            for half in range(2):
                ps, pb = nextps()
                psb = ps.bitcast(BF16)
                for j in range(4):
                    c = half * 4 + j
                    tr(psb[:, j * 128:(j + 1) * 128], ub[:, c * 128:(c + 1) * 128], ident_b, [ubb, cb], pb)
                cp("act", uT[:, half * 4:half * 4 + 4, tI * 128:(tI + 1) * 128],
                   psb[:, 0:512].rearrange("p (j t) -> p j t", t=128), [pb], [buT])
        dma("sp", uTd[s], uT, [buT], [db["uTd"]])
        SC.barrier(); SC.mark(f"  uT{l}{s}")
        AR.off = mark

        mark = AR.off
        wsc = AR.alloc([3, 12]); bsc = AR.alloc([12]); bws = B("wsc")
        for k_ in range(3):
            dma_nc("sp", wsc[:, k_, :], IN["w_sconv"][l, k_].rearrange("(j p) -> p j", p=128), (), [bws])
        dma_nc("sp", bsc, IN["b_sconv"][l].rearrange("(j p) -> p j", p=128), (), [bws])
        wbp = Pool_([8, 128], BF16, 3)
        hzp = Pool_([S + 2], F32, 2)
        for t_, b_ in zip(hzp.t, hzp.b):
            memset("dve", t_[:, 0:1], 0.0, [b_])
            memset("dve", t_[:, S + 1:S + 2], 0.0, [b_])
        t1p = Pool_([S], F32, 2); czp = Pool_([S], BF16, 3); stp = Pool_([16, 128], BF16, 2)
        def load_hw(j):
            wt, wbf = wbp.next()
            load_w_block(wt, IN["w_in"][l], C_HV + j * 128, 128, wbf)
            return wt, wbf
        hq = [load_hw(0), load_hw(1)]

        def hy_a(j):
            wt, wbf = hq.pop(0)
            if j + 2 < 12:
                hq.append(load_hw(j + 2))
            hz, hzb = hzp.next()
            for tb in range(4):
                ps, pb = nextps()
                for c in range(8):
                    mm(ps, wt[:, c, :], uT[:, c, tb * 512:(tb + 1) * 512], c == 0, c == 7, [wbf, buT], pb)
                cp("act", hz[:, 1 + tb * 512:1 + (tb + 1) * 512], ps, [pb], [hzb])
            t1, t1b = t1p.next()
            ts("dve", t1, hz[:, 1:S + 1], wsc[:, 1, j:j + 1], bsc[:, j:j + 1], ALU.mult, ALU.add, [hzb, bws], [t1b])
            stt("dve", t1, hz[:, 0:S], wsc[:, 0, j:j + 1], t1, ALU.mult, ALU.add, [hzb, bws, t1b], [t1b])
            cz, czb = czp.next()
            stt("dve", cz, hz[:, 2:S + 2], wsc[:, 2, j:j + 1], t1, ALU.mult, ALU.add, [hzb, bws, t1b], [czb])
            return cz, czb

        def hy_b(j, cz, czb):
            st_, stb = stp.next()
            for q4 in range(4):
                ps, pb = nextps()
                psb = ps.bitcast(BF16)
                for jj in range(4):
                    tc = q4 * 4 + jj
                    tr(psb[:, jj * 128:(jj + 1) * 128], cz[:, tc * 128:(tc + 1) * 128], ident_b, [czb, cb], pb)
                cp("act", st_[:, q4 * 4:q4 * 4 + 4, :], psb[:, 0:512].rearrange("p (j t) -> p j t", t=128),
                   [pb], [stb])
            which, cc = j // 4, j % 4
            dma_nc("sp", cTd[which, :, :, s * 512 + cc * 128:s * 512 + (cc + 1) * 128].rearrange("tc p c -> p tc c"),
                   st_, [stb], [db["cTd"]])

        cur = hy_a(0)
        for j in range(12):
            nxt_ = hy_a(j + 1) if j + 1 < 12 else None
            hy_b(j, *cur)
            cur = nxt_
        SC.barrier(); SC.mark(f"  hyproj{l}{s}")
        AR.off = mark

        mark = AR.off
        amask = AR.alloc([12, 256]); bam = B("amask")
        dma("sp", amask, CN["amask"], (), [bam])
        qT2 = AR.alloc([S], BF16); kT2 = AR.alloc([S], BF16); bq = B("q"); bk = B("k")
        v2 = AR.alloc([16, 130], BF16); bv = B("v2")
        memset("dve", v2[:, :, 64:65], 1.0, [bv])
        memset("dve", v2[:, :, 129:130], 1.0, [bv])
        accs = [AR.alloc([S]), AR.alloc([S])]; bacc = [B("acc0"), B("acc1")]
        rec = AR.alloc([S]); brec = B("rec")
        yaT = AR.alloc([4, S], BF16); bya = B("yaT")
        wqp = Pool_([8, 128], BF16, 6)
        Ep = Pool_([256], F32, 3); Pp = Pool_([256], BF16, 5)
        def load_qkv(hp, g):
            blk = 2 * g + hp
            wq, wqb = wqp.next(); wk, wkb = wqp.next(); wv, wvb = wqp.next()
            load_w_block(wq, IN["w_in"][l], C_Q + blk * 128, 128, wqb)
            load_w_block(wk, IN["w_in"][l], C_K + blk * 128, 128, wkb)
            load_w_block(wv, IN["w_in"][l], C_V + blk * 128, 128, wvb)
            return (wq, wqb, wk, wkb, wv, wvb)
        combos = [(hp, g) for hp in range(2) for g in range(3)]
        wnext = load_qkv(*combos[0])
        for hp in range(2):
            for a_, b_ in zip(accs, bacc):
                memset("dve", a_[0:65, :], 0.0, [b_])
            for g, (win, dil) in enumerate(GROUPS):
                L = S // dil
                (wq, wqb, wk, wkb, wv, wvb) = wnext
                ci = combos.index((hp, g))
                if ci + 1 < len(combos):
                    wnext = load_qkv(*combos[ci + 1])
                for (wt, wbf, dstT, bd, scl) in ((wq, wqb, qT2, bq, 0.125), (wk, wkb, kT2, bk, None)):
                    for tb in range(4):
                        ps, pb = nextps()
                        for c in range(8):
                            mm(ps, wt[:, c, :], uT[:, c, tb * 512:(tb + 1) * 512], c == 0, c == 7, [wbf, buT], pb)
                        if dil == 1:
                            o_ap = dstT[:, tb * 512:(tb + 1) * 512]; i_ap = ps
                        else:
                            mper = 512 // dil
                            o_ap = dstT.rearrange("p (r m) -> p r m", r=dil)[:, :, tb * mper:(tb + 1) * mper]
                            i_ap = ps.rearrange("p (m r) -> p r m", r=dil)
                        if scl is None:
                            cp("act", o_ap, i_ap, [pb], [bd])
                        else:
                            act(o_ap, i_ap, AF.Copy, [pb], [bd], scale=scl)
                nkt = L // 128
                for rho in range(dil):
                    for kt in range(nkt):
                        ti = rho * nkt + kt
                        ps, pb = nextps()
                        p0 = rho + dil * 128 * kt
                        for c in range(8):
                            if dil == 1:
                                lw = uT[:, c, p0:p0 + 128]
                            else:
                                lw = uT[:, c, p0:p0 + 127 * dil + 1:dil]
                            mm(ps[:, 0:128], lw, wv[:, c, :], c == 0, c == 7, [buT, wvb], pb)
                        cp("act", v2[:, ti, :].rearrange("p (h e) -> p h e", e=65)[:, :, 0:64],
                           ps[:, 0:128].rearrange("p (h e) -> p h e", e=64), [pb], [bv])
                def stage_a(rho, kt, h_, L=L, dil=dil, nkt=nkt, g=g):
                    qlo = max(0, 128 * kt - 64); qhi = min(L, 128 * kt + 192); nq = qhi - qlo
                    coff = qlo - (128 * kt - 64)
                    head = 4 * g + 2 * hp + h_
                    ps, pb = nextps()
                    mm(ps[:, 0:nq], kT2[64 * h_:64 * h_ + 64, rho * L + 128 * kt:rho * L + 128 * kt + 128],
                       qT2[64 * h_:64 * h_ + 64, rho * L + qlo:rho * L + qhi], True, True, [bk, bq], pb)
                    E, Eb = Ep.next()
                    act(E[:, 0:nq], ps[:, 0:nq], AF.Exp, [pb], [Eb])
                    P, Pb = Pp.next()
                    tt("pool", P[:, 0:nq], E[:, 0:nq], amask[:, head, coff:coff + nq], ALU.mult, [Eb, bam], [Pb])
                    return (P, Pb, qlo, qhi, nq)

                def stage_b(rho, kt, h_, a, L=L, dil=dil, nkt=nkt):
                    (P, Pb, qlo, qhi, nq) = a
                    ti = rho * nkt + kt
                    ps2, pb2 = nextps()
                    mm(ps2[0:65, 0:nq], v2[:, ti, 65 * h_:65 * h_ + 65], P[:, 0:nq], True, True, [bv, Pb], pb2)
                    if dil == 1:
                        av = accs[h_][0:65, qlo:qhi]
                    else:
                        av = accs[h_][0:65, :].rearrange("p (m r) -> p r m", r=dil)[:, rho, qlo:qhi]
                    tt("dve", av, av, ps2[0:65, 0:nq], ALU.add, [pb2, bacc[h_]], [bacc[h_]])

                iters = [(rho, kt, h_) for rho in range(dil) for kt in range(nkt) for h_ in range(2)]
                pend = []
                for it in iters:
                    pend.append((it, stage_a(*it)))
                    if len(pend) > 2:
                        itb, a_ = pend.pop(0)
                        stage_b(*itb, a_)
                for itb, a_ in pend:
                    stage_b(*itb, a_)
            for h_ in range(2):
                SC.op("dve", lambda e, h_=h_: e.reciprocal(out=rec[64:65, :], in_=accs[h_][64:65, :]),
                      [bacc[h_]], [brec])
                for tb in range(4):
                    ps, pb = nextps()
                    mm(ps[0:64, :], ones_f[64:65, 0:64], rec[64:65, tb * 512:(tb + 1) * 512], True, True, [cb, brec], pb)
                    tt("dve", yaT[0:64, 2 * hp + h_, tb * 512:(tb + 1) * 512], accs[h_][0:64, tb * 512:(tb + 1) * 512],
                       ps[0:64, :], ALU.mult, [pb, bacc[h_]], [bya])
        dma("sp", yaTd[s], yaT[0:64], [bya], [db["yaTd"]])
        SC.barrier(); SC.mark(f"  attn{l}{s}")
        AR.off = mark
        wbp = Pool_([8, 128], BF16, 3)

        PADW = S + 16
        ypb = AR.alloc([PADW]); byp = B("yp")
        wa = AR.alloc([PADW]); wb_ = AR.alloc([PADW]); bwa = B("wa"); bwb = B("wb")
        icr = AR.alloc([S]); bic = B("ic")
        pooled = AR.alloc([S], BF16); bpo = B("pooled")
        ybT = AR.alloc([4, S], BF16); byb = B("ybT")
        wpl = AR.alloc([4, 128], BF16); bwp = B("wpool")
        dma("pool", wpl, IN["w_pool"][l].rearrange("g c d -> c g d"), (), [bwp])
        pscale = AR.alloc([4]);
        dma_nc("sp", pscale, IN["pool_scale"][l].rearrange("(g p) -> p g", p=128), (), [bwp])
        memset("dve", ypb[:, 0:8], 0.0, [byp]); memset("dve", ypb[:, S + 8:S + 16], 0.0, [byp])
        def load_pw(g):
            wt, wbf = wbp.next()
            load_w_block(wt, IN["w_in"][l], C_P + g * 128, 128, wbf)
            return wt, wbf
        pq = [load_pw(0), load_pw(1)]
        for g, wd in enumerate((2, 4, 8, 16)):
            wt, wbf = pq.pop(0)
            if g + 2 < 4:
                pq.append(load_pw(g + 2))
            for tb in range(4):
                ps, pb = nextps()
                for c in range(8):
                    mm(ps, wt[:, c, :], uT[:, c, tb * 512:(tb + 1) * 512], c == 0, c == 7, [wbf, buT], pb)
                cp("act", ypb[:, 8 + tb * 512:8 + (tb + 1) * 512], ps, [pb], [byp])
            load_rep(icr, CN["invcnt"][g], [bic], bic)
            tt("dve", wa[:, 1:S + 15], ypb[:, 0:S + 14], ypb[:, 1:S + 15], ALU.add, [byp], [bwa])
            cur, bcur, oth, both = wa, bwa, wb_, bwb
            lo, hi = 1, S + 15
            step = 1
            for lev in range(g):
                nlo, nhi = lo + step, hi - step
                tt("dve", oth[:, nlo:nhi], cur[:, nlo - step:nhi - step], cur[:, nlo + step:nhi + step], ALU.add,
                   [bcur], [both])
                cur, bcur, oth, both = oth, both, cur, bcur
                lo, hi = nlo, nhi
                step *= 2
            tt("dve", oth[:, 8:S + 8], cur[:, 8:S + 8], icr, ALU.mult, [bcur, bic], [both])
            tt("dve", pooled, oth[:, 8:S + 8], ypb[:, 8:S + 8], ALU.subtract, [both, byp], [bpo])
            for tb in range(4):
                ps, pb = nextps()
                mm(ps, wpl[:, g, :], pooled[:, tb * 512:(tb + 1) * 512], True, True, [bwp, bpo], pb)
                act(ybT[:, g, tb * 512:(tb + 1) * 512], ps, AF.Copy, [pb, bwp], [byb], scale=pscale[:, g:g + 1])
        dma("sp", ybTd[s], ybT, [byb], [db["ybTd"]])

    def phase_hyena(l):
        AR.reset()
        zin = AR.alloc([16, 1024], BF16); bz = B("zin")
        dma("sp", zin, cTd[0].rearrange("tc p c -> p tc c"), [db["cTd"]], [bz])
        Y = AR.alloc([32, 1024], BF16); bY = B("Y")
        z2 = AR.alloc([16, 1024], BF16); bz2 = B("z2")
        fpool = Pool_([16, 128], BF16, 4)
        gpool = Pool_([32, 128], BF16, 2)
        kp = Pool_([512], F32, 4)
        tp = Pool_([1024], F32, 4)
        xp = Pool_([1024], BF16, 2)
        ycp = Pool_([1024], BF16, 2)
        ystage = Pool_([8, 128], BF16, 2)
        for o in range(2):
            src = zin if o == 0 else z2
            bsrc = bz if o == 0 else bz2
            def load_fc(fc, o=o):
                fts = []
                for part in range(2):
                    ft, fb = fpool.next()
                    dma("sp", ft, CN["dftF"][fc + 16 * part], (), [fb])
                    fts.append((ft, fb))
                kr, krb = kp.next(); ki, kib = kp.next()
                dma("sp", kr, kspec[fc, :, o * 512:(o + 1) * 512], [db["kspec"]], [krb])
                dma("sp", ki, kspec[fc + 16, :, o * 512:(o + 1) * 512], [db["kspec"]], [kib])
                return (fts, kr, krb, ki, kib)
            fnext = load_fc(0)
            for fc in range(16):
                (fts, kr, krb, ki, kib) = fnext
                if fc + 1 < 16:
                    fnext = load_fc(fc + 1)
                zr = []; zi = []
                for part in range(2):
                    ft, fb = fts[part]
                    for hh in range(2):
                        ps, pb = nextps()
                        for tc in range(16):
                            mm(ps, ft[:, tc, :], src[:, tc, hh * 512:(hh + 1) * 512], tc == 0, tc == 15, [fb, bsrc], pb)
                        (zr if part == 0 else zi).append((ps, pb))
                for hh in range(2):
                    (pr, prb), (pi_, pib) = zr[hh], zi[hh]
                    sl = slice(hh * 512, (hh + 1) * 512)
                    t1, t1b = tp.next(); t2, t2b = tp.next()
                    tt("dve", t1[:, 0:512], pr, kr, ALU.mult, [prb, krb], [t1b])
                    tt("dve", t2[:, 0:512], pi_, ki, ALU.mult, [pib, kib], [t2b])
                    tt("pool", Y[:, fc, sl], t1[:, 0:512], t2[:, 0:512], ALU.subtract, [t1b, t2b], [bY])
                    tt("dve", t1[:, 512:1024], pr, ki, ALU.mult, [prb, kib], [t1b])
                    tt("dve", t2[:, 512:1024], pi_, kr, ALU.mult, [pib, krb], [t2b])
                    tt("pool", Y[:, fc + 16, sl], t1[:, 512:1024], t2[:, 512:1024], ALU.add, [t1b, t2b], [bY])
                    if fc == 0:
                        tt("dve", Y[0:1, 0, sl], pr[0:1, :], kr[0:1, :], ALU.mult, [prb, krb, bY], [bY])
                        tt("dve", Y[0:1, 16, sl], pi_[0:1, :], ki[0:1, :], ALU.mult, [pib, kib, bY], [bY])
            def load_tc(tc, o=o):
                gt, gb = gpool.next()
                dma("sp", gt, CN["dftG"][tc], (), [gb])
                xt, xb = xp.next()
                dma("sp", xt, cTd[1 + o, tc], [db["cTd"]], [xb])
                return (gt, gb, xt, xb)
            tnext = load_tc(0)
            for tc in range(16):
                (gt, gb, xt, xb) = tnext
                if tc + 1 < 16:
                    tnext = load_tc(tc + 1)
                pss = []
                for hh in range(2):
                    ps, pb = nextps()
                    for fc in range(32):
                        mm(ps, gt[:, fc, :], Y[:, fc, hh * 512:(hh + 1) * 512], fc == 0, fc == 31, [gb, bY], pb)
                    pss.append((ps, pb))
                if o == 0:
                    for hh in range(2):
                        ps, pb = pss[hh]
                        tt("dve", z2[:, tc, hh * 512:(hh + 1) * 512], ps, xt[:, hh * 512:(hh + 1) * 512], ALU.mult,
                           [pb, xb], [bz2])
                else:
                    yc, ycb = ycp.next()
                    for hh in range(2):
                        ps, pb = pss[hh]
                        tt("dve", yc[:, hh * 512:(hh + 1) * 512], ps, xt[:, hh * 512:(hh + 1) * 512], ALU.mult,
                           [pb, xb], [ycb])
                    ys, ysb = ystage.next()
                    for hh in range(2):
                        ps, pb = nextps()
                        psb = ps.bitcast(BF16)
                        for cc in range(4):
                            tr(psb[:, cc * 128:(cc + 1) * 128], yc[:, hh * 512 + cc * 128:hh * 512 + (cc + 1) * 128],
                               ident_b, [ycb, cb], pb)
                        cp("act", ys[:, hh * 4:hh * 4 + 4, :], psb[:, 0:512].rearrange("p (j t) -> p j t", t=128),
                           [pb], [ysb])
                    for s_ in range(NB):
                        dma_nc("sp", ycTd[s_, :, :, tc * 128:(tc + 1) * 128], ys[:, s_ * 4:s_ * 4 + 4, :], [ysb],
                               [db["ycTd"]])

    def layer_norm_tile(r, rb, gam, bet, bprm, outt, outb, stp, mvp, geng="pool"):
        st_, stb = stp.next()
        for hh in range(2):
            SC.op("dve", lambda e, hh=hh: e.bn_stats(out=st_[:, hh * 6:(hh + 1) * 6], in_=r[:, hh * 512:(hh + 1) * 512]),
                  [rb], [stb])
        mv, mvb = mvp.next()
        SC.op("dve", lambda e: e.bn_aggr(out=mv[:, 0:2], in_=st_[:, 0:12]), [stb], [mvb])
        act(mv[:, 2:3], mv[:, 1:2], AF.Sqrt, [mvb, cb], [mvb], bias=epsb[:, 0:1])
        SC.op("dve", lambda e: e.reciprocal(out=mv[:, 3:4], in_=mv[:, 2:3]), [mvb], [mvb])
        stt("dve", mv[:, 2:3], mv[:, 0:1], -1.0, mv[:, 3:4], ALU.mult, ALU.mult, [mvb], [mvb])
        act(r, r, AF.Identity, [rb, mvb], [rb], bias=mv[:, 2:3], scale=mv[:, 3:4])
        tt(geng, r, r, gam, ALU.mult, [rb, bprm], [rb])
        tt("dve", outt, r, bet, ALU.add, [rb, bprm], [outb])

    def phase_merge(l, s, zero_cum):
        AR.reset()
        mergedT = AR.alloc([8, S], BF16); bmg = B("merged")
        mark = AR.off
        uT = AR.alloc([8, S], BF16); buT = B("uT")
        dma("sp", uT, uTd[s], [db["uTd"]], [buT])
        yaT = AR.alloc([4, S], BF16); ybT = AR.alloc([4, S], BF16); ycT = AR.alloc([4, S], BF16); bys = B("ys")
        dma("sp", yaT[0:64], yaTd[s], [db["yaTd"]], [bys])
        dma("sp", ybT, ybTd[s], [db["ybTd"]], [bys])
        dma("sp", ycT, ycTd[s], [db["ycTd"]], [bys])
        wgp = Pool_([8, 128], BF16, 6)
        pap = Pool_([4, 128], BF16, 2); ppp = Pool_([4, 128], BF16, 2); php = Pool_([4, 128], BF16, 2)
        Gp = Pool_([512], F32, 3); Mp = Pool_([512], F32, 2); Tp = Pool_([512], F32, 2)
        def load_dc(dc):
            wgs = []
            for i in range(3):
                wt, wbf = wgp.next()
                load_w_block(wt, IN["w_in"][l], C_G + i * D + dc * 128, 128, wbf)
                wgs.append((wt, wbf))
            pa, pab = pap.next(); pp, ppb = ppp.next(); ph, phb = php.next()
            dma("pool", pa[0:64], IN["p_attn"][l].rearrange("(h p) n -> p h n", p=64)[:, :, dc * 128:(dc + 1) * 128],
                (), [pab])
            load_w_block(pp, IN["p_pool"][l], dc * 128, 128, ppb)
            load_w_block(ph, IN["p_hyena"][l], dc * 128, 128, phb)
            return (wgs, pa, pab, pp, ppb, ph, phb)
        dnext = load_dc(0)
        for dc in range(8):
            (wgs, pa, pab, pp, ppb, ph, phb) = dnext
            if dc + 1 < 8:
                dnext = load_dc(dc + 1)
            for tb in range(4):
                tsl = slice(tb * 512, (tb + 1) * 512)
                m_, mb = Mp.next()
                for i in range(3):
                    wt, wbf = wgs[i]
                    ps, pb = nextps()
                    for c in range(8):
                        mm(ps, wt[:, c, :], uT[:, c, tsl], c == 0, c == 7, [wbf, buT], pb)
                    G, Gb = Gp.next()
                    act(G, ps, AF.Sigmoid, [pb], [Gb])
                    ps2, pb2 = nextps()
                    if i == 0:
                        for h_ in range(4):
                            mm(ps2, pa[0:64, h_, :], yaT[0:64, h_, tsl], h_ == 0, h_ == 3, [pab, bys], pb2)
                    else:
                        wsrc, wsb, ysrc = (pp, ppb, ybT) if i == 1 else (ph, phb, ycT)
                        for c in range(4):
                            mm(ps2, wsrc[:, c, :], ysrc[:, c, tsl], c == 0, c == 3, [wsb, bys], pb2)
                    if i == 0:
                        tt("dve", m_, G, ps2, ALU.mult, [Gb, pb2], [mb])
                    elif i == 1:
                        t_, tb_ = Tp.next()
                        tt("dve", t_, G, ps2, ALU.mult, [Gb, pb2], [tb_])
                        tt("pool", m_, m_, t_, ALU.add, [mb, tb_], [mb])
                    else:
                        t_, tb_ = Tp.next()
                        tt("dve", t_, G, ps2, ALU.mult, [Gb, pb2], [tb_])
                        tt("pool", mergedT[:, dc, tsl], m_, t_, ALU.add, [mb, tb_], [bmg])
        SC.barrier(); SC.mark(f"  mergedc{l}{s}")
        AR.off = mark
        wo = AR.alloc([8, D], BF16); bwo = B("wo")
        load_w_block(wo, IN["w_out"][l], 0, D, bwo)
        reps = {}
        brp = B("reps")
        for nm, row in (("g1", modd[l, s, 2 * D:3 * D]), ("sh2", modd[l, s, 3 * D:4 * D]), ("sc2", modd[l, s, 4 * D:5 * D]),
                        ("lg", IN["ln1_g"][l]), ("lb", IN["ln1_b"][l])):
            reps[nm] = AR.alloc([D])
            load_rep(reps[nm], row, [db["modd"]], brp)
        wr = AR.alloc([8, NE]); brt = AR.alloc([NE])
        dma_nc("sp", wr, IN["w_router"][l].rearrange("(c p) e -> p c e", p=128), (), [brp])
        load_rep(brt, IN["b_router"][l], (), brp)
        xin = IN["x"][s] if l == 0 else xcur[s * S:(s + 1) * S, :]
        xin_reads = [] if l == 0 else [db["xcur"]]
        xp = Pool_([D], F32, 4); rp = Pool_([D], F32, 4); x1p = Pool_([D], F32, 4); u2p = Pool_([D], F32, 4)
        u2Tp = Pool_([8, 128], F32, 4)
        u2b_all = [AR.alloc([D], BF16) for _ in range(16)]; u2bb_all = [B(f"u2b{i}") for i in range(16)]
        lg_all = AR.alloc([16, NE]); lgb_all = [B(f"lg{i}") for i in range(16)]
        saved = {}
        stp = Pool_([12], F32, 8); mvp = Pool_([4], F32, 8)
        smp = Pool_([NE], F32, 40); s8p = Pool_([8], F32, 16); ohp = Pool_([NE], F32, 16)
        cumsave = persist_cum

        def tile_gen(tI):
            gI = s * 16 + tI
            xt, xb = xp.next()
            dma("sp", xt, xin[tI * 128:(tI + 1) * 128, :], xin_reads, [xb])
            r, rb = rp.next()
            for hh in range(2):
                ps, pb = nextps()
                for c in range(8):
                    mm(ps, mergedT[:, c, tI * 128:(tI + 1) * 128], wo[:, c, hh * 512:(hh + 1) * 512], c == 0, c == 7,
                       [bmg, bwo], pb)
                tt("dve", r[:, hh * 512:(hh + 1) * 512], ps, reps["g1"][:, hh * 512:(hh + 1) * 512], ALU.mult,
                   [pb, brp], [rb])
            stt("dve", r, xt, ALPHA, r, ALU.mult, ALU.add, [xb, rb], [rb])
            yield
            x1, x1b = x1p.next()
            layer_norm_tile(r, rb, reps["lg"], reps["lb"], brp, x1, x1b, stp, mvp, geng="dve")
            dma("sp", x1d[gI * 128:(gI + 1) * 128, :], x1, [x1b], [db["x1d"]])
            yield
            u2, u2b_ = u2p.next()
            tt("dve", u2, x1, reps["sc2"], ALU.mult, [x1b, brp], [u2b_])
            tt("dve", u2, u2, reps["sh2"], ALU.add, [u2b_, brp], [u2b_])
            u2b = u2b_all[tI]; u2bb = u2bb_all[tI]
            cp("act", u2b, u2, [u2b_], [u2bb])
            yield
            u2T, u2Tb = u2Tp.next()
            for half in range(2):
                ps, pb = nextps()
                for j in range(4):
                    c = half * 4 + j
                    tr(ps[:, j * 128:(j + 1) * 128], u2[:, c * 128:(c + 1) * 128], ident_f, [u2b_, cb], pb)
                cp("act", u2T[:, half * 4:half * 4 + 4, :], ps.rearrange("p (j t) -> p j t", t=128), [pb], [u2Tb])
            ps, pb = nextps()
            for c in range(8):
                mm(ps[:, 0:NE], u2T[:, c, :], wr[:, c, :], c == 0, c == 7, [u2Tb, brp], pb)
            lg = lg_all[:, tI, :]; lgb = lgb_all[tI]
            tt("dve", lg, ps[:, 0:NE], brt, ALU.add, [pb, brp], [lgb])
            saved[tI] = (u2b, u2bb)
            yield

        def tile_gen_b(tI):
            gI = s * 16 + tI
            lg = lg_all[:, tI, :]; lgb = lgb_all[tI]
            u2b, u2bb = saved[tI]
            if "dbg_log" in debug:
                dma("sp", dbg_log[gI * 128:(gI + 1) * 128, :], lg, [lgb], [db["dbg"]])
            t8, t8b = s8p.next()
            SC.op("dve", lambda e, t8=t8, lg=lg: e.max(out=t8, in_=lg), [lgb], [t8b])
            mask, mkb = smp.next()
            ts("dve", mask, lg, t8[:, 3:4], None, ALU.is_ge, None, [lgb, t8b], [mkb])
            yield
            nmx, nmxb = s8p.next()
            ts("dve", nmx[:, 0:1], t8[:, 0:1], -1.0, None, ALU.mult, None, [t8b], [nmxb])
            ex, exb = smp.next()
            act(ex, lg, AF.Exp, [lgb, nmxb], [exb], bias=nmx[:, 0:1])
            tt("dve", ex, ex, mask, ALU.mult, [exb, mkb], [exb])
            yield
            SC.op("dve", lambda e, nmx=nmx, ex=ex: e.reduce_sum(out=nmx[:, 1:2], in_=ex, axis=AX.X), [exb, nmxb], [nmxb])
            SC.op("dve", lambda e, nmx=nmx: e.reciprocal(out=nmx[:, 2:3], in_=nmx[:, 1:2]), [nmxb], [nmxb])
            gt_, gtb = smp.next()
            ts("dve", gt_, ex, nmx[:, 2:3], None, ALU.mult, None, [exb, nmxb], [gtb])
            yield
            ps, pb = nextps()
            mm(ps[:, 0:NE], ltri, mask, True, False, [cb, mkb], pb)
            mm(ps[:, 0:NE], ones_f, cumsave, False, True, [cb, b_cum], pb)
            tt("dve", cumsave, cumsave, mask, ALU.add, [mkb, b_cum], [b_cum])
            dst, dstb = smp.next()
            vl, vlb = smp.next()
            ts("dve", vl, ps[:, 0:NE], float(CAP) - 0.5, None, ALU.is_lt, None, [pb], [vlb])
            tt("dve", vl, vl, mask, ALU.mult, [vlb, mkb], [vlb])
            yield
            tt("dve", dst, ps[:, 0:NE], eoff, ALU.add, [pb, cb], [dstb])
            ts("dve", dst, dst, -BIG, None, ALU.add, None, [dstb], [dstb])
            tt("dve", dst, dst, vl, ALU.mult, [dstb, vlb], [dstb])
            yield
            ts("dve", dst, dst, BIG, None, ALU.add, None, [dstb], [dstb])
            dk, dkb = s8p.next()
            yield
            for k in range(4):
                oh, ohb = ohp.next()
                ts("dve", oh, lg, t8[:, k:k + 1], None, ALU.is_equal, None, [lgb, t8b], [ohb])
                o2, o2b = ohp.next()
                tt("dve", o2, oh, dst, ALU.mult, [ohb, dstb], [o2b])
                SC.op("dve", lambda e, dk=dk, o2=o2, k=k: e.reduce_sum(out=dk[:, k:k + 1], in_=o2, axis=AX.X),
                      [o2b, dkb], [dkb])
                tt("dve", o2, oh, gt_, ALU.mult, [ohb, gtb, o2b], [o2b])
                SC.op("dve", lambda e, o2=o2, k=k, gI=gI: e.reduce_sum(out=gate_all[:, gI, k:k + 1], in_=o2, axis=AX.X),
                      [o2b, b_route_t[gI]], [b_route_t[gI]])
                yield
            cp("dve", dest_all[:, gI * 4:gI * 4 + 4], dk[:, 0:4], [dkb, b_route_t[gI]], [b_route_t[gI]])
            if "dbg_dest" in debug:
                dma("sp", dbg_dest[gI * 128:(gI + 1) * 128, :], dk[:, 0:4], [dkb], [db["dbg"]])
            for k in range(4):
                SC.op("pool", lambda e, u2b=u2b, gI=gI, k=k: e.indirect_dma_start(
                    out=xe_d, out_offset=bass.IndirectOffsetOnAxis(ap=dest_all[:, gI * 4 + k:gI * 4 + k + 1], axis=0),
                    in_=u2b, in_offset=None, bounds_check=holder["bc"], oob_is_err=False),
                    [u2bb, b_route_t[gI], db["xe_d"]], [db["xe_d"]], dma=True)

        def drive(gens_, stag, maxact):
            active = []
            nxt_t = 0
            step_ = 0
            while nxt_t < len(gens_) or active:
                if nxt_t < len(gens_) and step_ % stag == 0 and len(active) < maxact:
                    active.append(gens_[nxt_t])
                    nxt_t += 1
                for g_ in list(active):
                    try:
                        next(g_)
                    except StopIteration:
                        active.remove(g_)
                step_ += 1
        drive([tile_gen(tI) for tI in range(16)], 2, 3)
        drive([tile_gen_b(tI) for tI in range(16)], 3, 4)

    def phase_zero_xe():
        AR.reset()
        z = AR.alloc([8192], BF16); bzz = B("z")
        memset("dve", z, 0.0, [bzz])
        rows_per = 128 * 8
        for i in range(NROWS // rows_per):
            dma("sp", xe_d[i * rows_per:(i + 1) * rows_per, :].rearrange("(p a) d -> p (a d)", p=128), z, [bzz],
                [db["xe_d"]])
        dma("sp", ye_d[NROWS:NROWS + 128, :], z[:, 0:D], [bzz], [db["ye_d"]])

    def phase_experts(l):
        AR.reset()
        w1p = Pool_([8, 2 * D], BF16, 2); w2p = Pool_([8, D], BF16, 2)
        b1p = Pool_([16], F32, 2); b2p = Pool_([D], F32, 2)
        xrp = Pool_([D], BF16, 2)
        xeTp = Pool_([8, CAP], BF16, 2); actTp = Pool_([8, CAP], BF16, 2)
        glp = Pool_([CAP], F32, 2); lnp = Pool_([CAP], F32, 2); sgp = Pool_([CAP], F32, 2)
        yp = Pool_([D], BF16, 3)
        NRT = CAP // 128
        pieces = [(lo_, min(lo_ + 512, CAP)) for lo_ in range(0, CAP, 512)]

        def stage_L(e_):
            w1t, w1b_ = w1p.next(); w2t, w2b_ = w2p.next(); b1t, b1b = b1p.next(); b2t, b2b = b2p.next()
            for c in range(8):
                dma("pool", w1t[:, c, :], IN["w1"][l, e_, c * 128:(c + 1) * 128, :], (), [w1b_])
            load_w_block(w2t, IN["w2"][l, e_], 0, D, w2b_)
            dma_nc("sp", b1t, IN["b1"][l, e_].rearrange("(j p) -> p j", p=128), (), [b1b])
            load_rep(b2t, IN["b2"][l, e_], (), b2b)
            return (w1t, w1b_, w2t, w2b_, b1t, b1b, b2t, b2b)

        def stage_T(e_):
            xeT, bxeT = xeTp.next()
            for rt in range(NRT):
                xr, xrb = xrp.next()
                dma("sp", xr, xe_d[e_ * CAP + rt * 128:e_ * CAP + (rt + 1) * 128, :], [db["xe_d"]], [xrb])
                for half in range(2):
                    ps, pb = nextps()
                    psb = ps.bitcast(BF16)
                    for j in range(4):
                        c = half * 4 + j
                        tr(psb[:, j * 128:(j + 1) * 128], xr[:, c * 128:(c + 1) * 128], ident_b, [xrb, cb], pb)
                    cp("act", xeT[:, half * 4:half * 4 + 4, rt * 128:(rt + 1) * 128],
                       psb[:, 0:512].rearrange("p (j t) -> p j t", t=128), [pb], [bxeT])
            return (xeT, bxeT)

        def stage_F(e_, W, X):
            (w1t, w1b_, w2t, w2b_, b1t, b1b, b2t, b2b) = W
            xeT, bxeT = X
            actT, bact = actTp.next()
            for fj in range(8):
                gl, glb = glp.next(); ln_, lnb = lnp.next(); sg, sgb = sgp.next()
                for (lo, hi) in pieces:
                    n_ = hi - lo
                    psg, pbg = nextps()
                    for c in range(8):
                        mm(psg[:, 0:n_], w1t[:, c, fj * 128:(fj + 1) * 128], xeT[:, c, lo:hi], c == 0, c == 7,
                           [w1b_, bxeT], pbg)
                    psl, pbl = nextps()
                    for c in range(8):
                        mm(psl[:, 0:n_], w1t[:, c, D + fj * 128:D + (fj + 1) * 128], xeT[:, c, lo:hi], c == 0, c == 7,
                           [w1b_, bxeT], pbl)
                    ts("dve", gl[:, lo:hi], psg[:, 0:n_], b1t[:, fj:fj + 1], 7.0, ALU.add, ALU.min, [pbg, b1b], [glb])
                    ts("dve", ln_[:, lo:hi], psl[:, 0:n_], b1t[:, 8 + fj:9 + fj], 7.0, ALU.add, ALU.min, [pbl, b1b], [lnb])
                act(sg, gl, AF.Sigmoid, [glb], [sgb], scale=1.702)
                ts("dve", ln_, ln_, -7.0, 1.0, ALU.max, ALU.add, [lnb], [lnb])
                tt("pool", sg, sg, gl, ALU.mult, [sgb, glb], [sgb])
                tt("pool", actT[:, fj, :], sg, ln_, ALU.mult, [sgb, lnb], [bact])
            return (actT, bact)

        def stage_G(e_, W, A):
            (w1t, w1b_, w2t, w2b_, b1t, b1b, b2t, b2b) = W
            actT, bact = A
            for rt in range(NRT):
                y, yb = yp.next()
                for hh in range(2):
                    ps, pb = nextps()
                    for c in range(8):
                        mm(ps, actT[:, c, rt * 128:(rt + 1) * 128], w2t[:, c, hh * 512:(hh + 1) * 512], c == 0, c == 7,
                           [bact, w2b_], pb)
                    if hh == 0:
                        tt("dve", y[:, hh * 512:(hh + 1) * 512], ps, b2t[:, hh * 512:(hh + 1) * 512], ALU.add,
                           [pb, b2b], [yb])
                    else:
                        tt("dve", y[:, hh * 512:(hh + 1) * 512], ps, b2t[:, hh * 512:(hh + 1) * 512], ALU.add,
                           [pb, b2b], [yb])
                dma("sp", ye_d[e_ * CAP + rt * 128:e_ * CAP + (rt + 1) * 128, :], y, [yb], [db["ye_d"]])

        Wd = {0: stage_L(0)}
        Xd = {0: stage_T(0)}
        for e_ in range(NE):
            if e_ + 1 < NE:
                Wd[e_ + 1] = stage_L(e_ + 1)
            A = stage_F(e_, Wd[e_], Xd[e_])
            if e_ + 1 < NE:
                Xd[e_ + 1] = stage_T(e_ + 1)
            stage_G(e_, Wd[e_], A)

    def phase_combine(l, last):
        AR.reset()
        gp = Pool_([D], BF16, 20); ap_ = Pool_([D], F32, 4); xp = Pool_([D], F32, 5); op_ = Pool_([D], F32, 4)
        stp = Pool_([12], F32, 4); mvp = Pool_([4], F32, 4)
        brp = B("reps")
        lg = AR.alloc([D]); lb = AR.alloc([D])
        load_rep(lg, IN["ln2_g"][l], (), brp)
        load_rep(lb, IN["ln2_b"][l], (), brp)
        g2 = [AR.alloc([D]) for _ in range(NB)]
        for s in range(NB):
            load_rep(g2[s], modd[l, s, 5 * D:6 * D], [db["modd"]], brp)

        def loads(gI):
            gs = []
            for k in range(4):
                g, gb = gp.next()
                SC.op("pool", lambda e, g=g, gI=gI, k=k: e.indirect_dma_start(
                    out=g, out_offset=None, in_=ye_d,
                    in_offset=bass.IndirectOffsetOnAxis(ap=dest_all[:, gI * 4 + k:gI * 4 + k + 1], axis=0),
                    bounds_check=holder["bc"], oob_is_err=False), [gb, b_route_t[gI], db["ye_d"]], [gb], dma=True)
                gs.append((g, gb))
            xt, xb = xp.next()
            dma("sp", xt, x1d[gI * 128:(gI + 1) * 128, :], [db["x1d"]], [xb])
            return gs, xt, xb

        NT = T // 128
        loaded = {0: loads(0), 1: loads(1), 2: loads(2)}

        def ctile(gI):
            s = gI // 16
            gs, xt, xb = loaded.pop(gI)
            if gI + 3 < NT:
                loaded[gI + 3] = loads(gI + 3)
            acc, accb = ap_.next()
            for k in range(4):
                g, gb = gs[k]
                if k == 0:
                    act(acc, g, AF.Copy, [gb, b_route_t[gI]], [accb], scale=gate_all[:, gI, 0:1])
                else:
                    stt("dve", acc, g, gate_all[:, gI, k:k + 1], acc, ALU.mult, ALU.add, [gb, b_route_t[gI], accb], [accb])
                yield
            tt("dve", acc, acc, g2[s], ALU.mult, [accb, brp], [accb])
            yield
            stt("dve", acc, xt, ALPHA, acc, ALU.mult, ALU.add, [xb, accb], [accb])
            yield
            o, ob = op_.next()
            layer_norm_tile(acc, accb, lg, lb, brp, o, ob, stp, mvp)
            if last:
                dma("sp", out_d.rearrange("b s d -> (b s) d")[gI * 128:(gI + 1) * 128, :], o, [ob], [db["out"]])
            else:
                dma("sp", xcur[gI * 128:(gI + 1) * 128, :], o, [ob], [db["xcur"]])

        cg = [ctile(gI) for gI in range(NT)]
        for a_ in range(0, NT, 2):
            active = [cg[a_], cg[a_ + 1]]
            while active:
                for g_ in list(active):
                    try:
                        next(g_)
                    except StopIteration:
                        active.remove(g_)

    persist = {}
    persist_cum = ltri
    b_cum = B("cum")

    AR.reset()
    persist_cum = AR.alloc([NE])
    AR.mark_persistent()

    def run():
        SC.mark("start")
        phase_ada(); SC.barrier(); SC.mark("ada")
        if stop_after == "ada":
            return
        phase_zero_xe(); SC.barrier(); SC.mark("zero")
        for l in range(n_layers):
            phase_filter(l); SC.barrier(); SC.mark(f"filter{l}")
            if stop_after == "filter":
                return
            for s in range(NB):
                phase_seq_front(l, s); SC.barrier(); SC.mark(f"front{l}{s}")
                if stop_after == "front0":
                    return
            phase_hyena(l); SC.barrier(); SC.mark(f"hyena{l}")
            if stop_after == "hyena":
                return
            memset("dve", persist_cum, 0.0, [b_cum])
            for s in range(NB):
                phase_merge(l, s, False); SC.barrier(); SC.mark(f"merge{l}{s}")
                if stop_after == "merge0":
                    return
            phase_experts(l); SC.barrier(); SC.mark(f"experts{l}")
            phase_combine(l, l == n_layers - 1); SC.barrier(); SC.mark(f"combine{l}")

    run()
    finals = [db[n] for n in ["out", "modd", "kspec", "uTd", "cTd", "yaTd", "ybTd", "ycTd", "x1d", "xcur", "xe_d",
                              "ye_d", "dbg"]]
    SC.finalize(finals)
    return nc, SC


_CONSTS = None
_PROG = None


def kernel(**inputs):
    global _CONSTS, _PROG
    if _CONSTS is None:
        _CONSTS = host_constants()
    if _PROG is None:
        _PROG = build_program()
    nc, _ = _PROG
    ncores = 8
    in_maps = []
    for r in range(ncores):
        m = {}
        for k in INPUT_SHAPES:
            a = np.asarray(inputs[k])
            if k in ("x", "c"):
                a = a[r * NB:(r + 1) * NB]
            m[k] = np.ascontiguousarray(a, dtype=np.float32)
        for k, v in _CONSTS.items():
            m[k] = v
        in_maps.append(m)
    res = run_bass_kernel_spmd(nc, in_maps, core_ids=list(range(ncores)))
    out = np.concatenate([np.asarray(r_["out"]) for r_ in res.results], axis=0)
    return out.astype(np.float32)
```
